# Optimizing a Trainium2 kernel written in Bass

```python
import jax, jax.numpy as jnp
from jax import lax
import numpy as np

D_MODEL = 2048
BATCH = 4
SEQ = 4096
DEPTH = 2

HEAD_DIM = 128
N_HEADS_DIL = D_MODEL // (2 * HEAD_DIM)
N_HEADS_SB = D_MODEL // (2 * HEAD_DIM)
D_DIL = N_HEADS_DIL * HEAD_DIM
D_SB = N_HEADS_SB * HEAD_DIM
D_MIX = D_DIL + D_SB
DILATED_BRANCHES = ((128, 1), (512, 4), (2048, 16))
Q_BLOCK = 128
N_BUCKETS = 32
MAX_DISTANCE = 2048
D_FF_DENSE = 5504
N_EXPERTS = 8
TOP_K = 2
D_FF_EXPERT = 7168
N_DENSE_LAYERS = (DEPTH + 1) // 2
N_MOE_LAYERS = DEPTH // 2
EPS = 1e-6

kernel_name = "hybrid_dilated_stickbreaking_moe_block"


def rms_norm(x, g):
    xf = x.astype(jnp.float32)
    y = xf * lax.rsqrt(jnp.mean(xf * xf, axis=-1, keepdims=True) + EPS)
    return (y * g.astype(jnp.float32)).astype(x.dtype)


def t5_bucket(dist):
    n = np.asarray(dist, dtype=np.int64)
    max_exact = N_BUCKETS // 2
    large = max_exact + (np.log(np.maximum(n, 1) / max_exact)
                         / np.log(MAX_DISTANCE / max_exact)
                         * (N_BUCKETS - max_exact)).astype(np.int64)
    large = np.minimum(large, N_BUCKETS - 1)
    return np.where(n < max_exact, n, large).astype(np.int32)


def to_blocks(t):
    b, h, s, dh = t.shape
    return t.reshape(b, h, s // Q_BLOCK, Q_BLOCK, dh).transpose(2, 0, 1, 3, 4)


def from_blocks(t):
    nb, b, h, q, dh = t.shape
    return t.transpose(1, 2, 0, 3, 4).reshape(b, h, nb * q, dh)


def dilated_attention(q, k, v, rel_bias):
    b, h, s, dh = q.shape
    nb = s // Q_BLOCK
    qb = to_blocks(q * (dh ** -0.5))
    branches = []
    for (w, d) in DILATED_BRANCHES:
        m = np.arange(w // d + 1, dtype=np.int32)
        bias = rel_bias[t5_bucket(d * m)].T.astype(jnp.float32)
        branches.append((d, m, bias))

    def block(args):
        q_blk, i = args
        t = i * Q_BLOCK + jnp.arange(Q_BLOCK, dtype=jnp.int32)
        lses, outs = [], []
        for d, m, bias in branches:
            idx = t[:, None] - d * m[None, :]
            valid = idx >= 0
            idx = jnp.maximum(idx, 0)
            k_g = k[:, :, idx]
            v_g = v[:, :, idx]
            sc = jnp.einsum('bhqd,bhqmd->bhqm', q_blk, k_g).astype(jnp.float32)
            sc = jnp.where(valid, sc + bias[None, :, None, :], -jnp.inf)
            lse = jax.nn.logsumexp(sc, axis=-1, keepdims=True)
            p = jnp.exp(sc - lse).astype(v.dtype)
            outs.append(jnp.einsum('bhqm,bhqmd->bhqd', p, v_g).astype(jnp.float32))
            lses.append(lse)
        wts = jax.nn.softmax(jnp.stack(lses, axis=0), axis=0)
        return jnp.sum(wts * jnp.stack(outs, axis=0), axis=0).astype(q.dtype)

    out = lax.map(block, (qb, jnp.arange(nb, dtype=jnp.int32)))
    return from_blocks(out)


def stick_breaking_attention(q, k, v):
    b, h, s, dh = q.shape
    nb = s // Q_BLOCK
    qb = to_blocks(q * (dh ** -0.5))
    pos = jnp.arange(s, dtype=jnp.int32)

    def block(args):
        q_blk, i = args
        t = i * Q_BLOCK + jnp.arange(Q_BLOCK, dtype=jnp.int32)
        causal = pos[None, :] < t[:, None]
        z = jnp.einsum('bhqd,bhsd->bhqs', q_blk, k).astype(jnp.float32)
        log_keep = jnp.where(causal, jax.nn.log_sigmoid(-z), 0.0)
        later = lax.cumsum(log_keep, axis=3, reverse=True) - log_keep
        a = jnp.where(causal, jnp.exp(jax.nn.log_sigmoid(z) + later), 0.0)
        return jnp.einsum('bhqs,bhsd->bhqd', a.astype(v.dtype), v)

    out = lax.map(block, (qb, jnp.arange(nb, dtype=jnp.int32)))
    return from_blocks(out)


def token_mixer(h, w_in, w_o, g_dil, g_sb, rel_bias):
    b, s, _ = h.shape
    proj = h @ w_in
    qd, kd, vd, qs, ks, vs = jnp.split(
        proj, [D_DIL, 2 * D_DIL, 3 * D_DIL, 3 * D_DIL + D_SB, 3 * D_DIL + 2 * D_SB], axis=-1)
    heads = lambda t, n: t.reshape(b, s, n, HEAD_DIM).transpose(0, 2, 1, 3)
    merge = lambda t: t.transpose(0, 2, 1, 3).reshape(b, s, -1)
    o_dil = merge(dilated_attention(heads(qd, N_HEADS_DIL), heads(kd, N_HEADS_DIL),
                                    heads(vd, N_HEADS_DIL), rel_bias))
    o_sb = merge(stick_breaking_attention(heads(qs, N_HEADS_SB), heads(ks, N_HEADS_SB),
                                          heads(vs, N_HEADS_SB)))
    o = jnp.concatenate([rms_norm(o_dil, g_dil), rms_norm(o_sb, g_sb)], axis=-1)
    return o @ w_o


def swiglu(h, w_gate, w_up, w_down):
    return (jax.nn.silu(h @ w_gate) * (h @ w_up)) @ w_down


def moe_swiglu(h, w_router, w_gate, w_up, w_down):
    logits = (h @ w_router).astype(jnp.float32)
    top_v, top_i = lax.top_k(logits, TOP_K)
    top_w = jax.nn.softmax(top_v, axis=-1)
    gates = jnp.sum(jax.nn.one_hot(top_i, N_EXPERTS, dtype=jnp.float32) * top_w[..., None], axis=-2)
    y = jnp.zeros_like(h)
    for e in range(N_EXPERTS):
        y = y + gates[..., e:e + 1].astype(h.dtype) * swiglu(h, w_gate[e], w_up[e], w_down[e])
    return y


def setup_inputs(seed: int = 0) -> dict:
    key = jax.random.key(seed)
    ks = jax.random.split(key, 18)
    nrm = lambda k, shape, scale: jax.random.normal(k, shape, jnp.float32) * scale
    gain = lambda k, shape: 1.0 + 0.02 * jax.random.normal(k, shape, jnp.float32)
    return {
        "x": nrm(ks[0], (BATCH, SEQ, D_MODEL), 1.0),
        "rel_bias": nrm(ks[1], (N_BUCKETS, N_HEADS_DIL), 0.5),
        "attn_norm_g": gain(ks[2], (DEPTH, D_MODEL)),
        "w_in": nrm(ks[3], (DEPTH, D_MODEL, 3 * D_MIX), D_MODEL ** -0.5),
        "mix_norm_dil_g": gain(ks[4], (DEPTH, D_DIL)),
        "mix_norm_sb_g": gain(ks[5], (DEPTH, D_SB)),
        "w_o": nrm(ks[6], (DEPTH, D_MIX, D_MODEL), D_MIX ** -0.5),
        "ffn_norm_g": gain(ks[7], (DEPTH, D_MODEL)),
        "dense_w_gate": nrm(ks[8], (N_DENSE_LAYERS, D_MODEL, D_FF_DENSE), D_MODEL ** -0.5),
        "dense_w_up": nrm(ks[9], (N_DENSE_LAYERS, D_MODEL, D_FF_DENSE), D_MODEL ** -0.5),
        "dense_w_down": nrm(ks[10], (N_DENSE_LAYERS, D_FF_DENSE, D_MODEL), D_FF_DENSE ** -0.5),
        "router_w": nrm(ks[11], (N_MOE_LAYERS, D_MODEL, N_EXPERTS), D_MODEL ** -0.5),
        "moe_w_gate": nrm(ks[12], (N_MOE_LAYERS, N_EXPERTS, D_MODEL, D_FF_EXPERT), D_MODEL ** -0.5),
        "moe_w_up": nrm(ks[13], (N_MOE_LAYERS, N_EXPERTS, D_MODEL, D_FF_EXPERT), D_MODEL ** -0.5),
        "moe_w_down": nrm(ks[14], (N_MOE_LAYERS, N_EXPERTS, D_FF_EXPERT, D_MODEL), D_FF_EXPERT ** -0.5),
        "final_norm_g": gain(ks[15], (D_MODEL,)),
    }


def reference(x, rel_bias, attn_norm_g, w_in, mix_norm_dil_g, mix_norm_sb_g, w_o, ffn_norm_g,
              dense_w_gate, dense_w_up, dense_w_down, router_w, moe_w_gate, moe_w_up,
              moe_w_down, final_norm_g):
    for layer in range(DEPTH):
        h = rms_norm(x, attn_norm_g[layer])
        x = x + token_mixer(h, w_in[layer], w_o[layer], mix_norm_dil_g[layer],
                            mix_norm_sb_g[layer], rel_bias)
        h = rms_norm(x, ffn_norm_g[layer])
        j = layer // 2
        if layer % 2 == 0:
            x = x + swiglu(h, dense_w_gate[j], dense_w_up[j], dense_w_down[j])
        else:
            x = x + moe_swiglu(h, router_w[j], moe_w_gate[j], moe_w_up[j], moe_w_down[j])
    return rms_norm(x, final_norm_g)
```

```python
import numpy as np
import concourse.bass as bass
import concourse.mybir as mybir
from concourse.bass_utils import run_bass_kernel_spmd

F32 = mybir.dt.float32
BF16 = mybir.dt.bfloat16
AF = mybir.ActivationFunctionType
ALU = mybir.AluOpType
AX = mybir.AxisListType

D = 2048
KC = 16
HD = 128
NH = 8
NCORE = 8
EPS = 1e-6
NBUCK = 32
TW = 3968
CPW = TW + 127
SAME_ENGINE_SYNC = True


class Cfg:
    def __init__(self, S=4096, FFD=5504, FFE=7168, depth=2):
        self.S = S; self.FFD = FFD; self.FFE = FFE; self.depth = depth
        self.NT = S
        self.NS = self.NT // 512
        self.NB = self.NT // 128


class Buf:
    __slots__ = ("name", "w", "r")

    def __init__(self, name):
        self.name = name; self.w = []; self.r = []


class Op:
    __slots__ = ("eng", "fn", "deps", "dma_sem", "sig", "val", "inc", "waits")

    def __init__(self, eng, fn, dma_sem, inc):
        self.eng = eng; self.fn = fn; self.deps = set(); self.dma_sem = dma_sem
        self.sig = False; self.val = 0; self.inc = inc; self.waits = {}


class Rec:
    def __init__(self):
        self.ops = []

    def add(self, eng, fn, reads=(), writes=(), dma_sem=None, inc=16):
        op = Op(eng, fn, dma_sem, inc)
        idx = len(self.ops)
        for b in reads:
            op.deps.update(b.w)
        for b in writes:
            if b.r:
                op.deps.update(b.r)
                op.deps.update(b.w)
                b.w = []; b.r = []
        for b in reads:
            b.r.append(idx)
        for b in writes:
            if b in reads:
                b.w = []; b.r = []
            b.w.append(idx)
        op.deps.discard(idx)
        self.ops.append(op)
        return idx

    def finalize(self):
        ops = self.ops
        for op in ops:
            for d in op.deps:
                ops[d].sig = True
        cnt = {}
        for op in ops:
            for d in op.deps:
                o = ops[d]
                if o.dma_sem is not None:
                    op.waits[o.dma_sem] = cnt[o.dma_sem]
                else:
                    if o.eng == op.eng and op.dma_sem is None and not SAME_ENGINE_SYNC:
                        continue
                    if o.val > op.waits.get(o.eng, 0):
                        op.waits[o.eng] = o.val
            if op.dma_sem is not None:
                cnt[op.dma_sem] = cnt.get(op.dma_sem, 0) + op.inc
                op.val = cnt[op.dma_sem]
            else:
                if op.sig:
                    cnt[op.eng] = cnt.get(op.eng, 0) + 1
                op.val = cnt.get(op.eng, 0)
        self.final = cnt

    def emit(self, eng_name, eng, sems):
        waited = {}
        for op in self.ops:
            if op.eng != eng_name:
                continue
            for key, v in op.waits.items():
                if waited.get(key, 0) < v:
                    eng.wait_ge(sems[key], v)
                    waited[key] = v
            ins = op.fn(eng)
            if op.dma_sem is not None:
                ins.then_inc(sems[op.dma_sem], op.inc)
            elif op.sig:
                ins.then_inc(sems[op.eng], 1)


def t5_bucket(n):
    n = np.asarray(n, dtype=np.int64)
    max_exact = NBUCK // 2
    large = max_exact + (np.log(np.maximum(n, 1) / max_exact) / np.log(2048 / max_exact)
                         * (NBUCK - max_exact)).astype(np.int64)
    large = np.minimum(large, NBUCK - 1)
    return np.where(n < max_exact, n, large).astype(np.int32)


def host_constants(par):
    X = np.arange(CPW)
    delta = X - 1023 + 128 * par
    n = ((delta <= 128).astype(np.int64) + ((delta % 4 == 0) & (delta <= 512)).astype(np.int64)
         + ((delta % 16 == 0) & (delta <= 2048)).astype(np.int64))
    n = np.where((delta >= 0) & (delta <= 2048), n, 0)
    bk = t5_bucket(np.clip(delta, 0, 2048))
    wt = np.zeros((NBUCK, CPW), np.float32)
    wt[bk, X] = n
    r = np.arange(8)[:, None, None, None]
    s = np.arange(128)[None, :, None, None]
    b = np.arange(4)[None, None, :, None]
    t = np.arange(128)[None, None, None, :]
    mask = ((128 * (b - r) + t - s) > 0).astype(np.float32).reshape(8, 128, 512)
    return wt, mask


def build(cfg):
    S, NT, NS, FFD, FFE = cfg.S, cfg.NT, cfg.NS, cfg.FFD, cfg.FFE
    nc = bass.Bass("TRN2", target_bir_lowering=False)
    R = Rec()
    L = cfg.depth

    def dram_in(name, shape, dt=F32):
        return nc.dram_tensor(name, list(shape), dt, kind="ExternalInput")

    def dram(name, shape, dt):
        return nc.dram_tensor(name, list(shape), dt)

    x_in = dram_in("x", [NT, D])
    R_IN, R_O, R_F = 256 * 3 * D // 2048, 256, 256 * FFD // 2048
    off_rows = {}
    _r = 0
    for l in range(L):
        off_rows[f"w_in{l}"] = _r; _r += R_IN
        off_rows[f"w_o{l}"] = _r; _r += R_O
    for nm in ("dg", "du", "dd"):
        off_rows[nm] = _r; _r += R_F
    NR = _r
    NRE = NR * 2048
    wsh_in = dram_in("wsh", [NR, 2048])
    eg_in = dram_in("eg", [D, FFE])
    eu_in = dram_in("eu", [D, FFE])
    ed_in = dram_in("ed", [FFE, D])
    gvec_in = dram_in("gvec", [128, 5 * KC])
    gmix_in = dram_in("gmix", [128, 2 * KC])
    relb_in = dram_in("rel_bias", [NBUCK, NH])
    rw_in = dram_in("router_w", [128, KC, 8])
    wt_in = dram_in("wt_c", [NBUCK, CPW])
    mask_in = dram_in("mask_c", [8, 128, 512])
    sel_in = dram_in("sel_c", [8, 128])
    ident_in = dram_in("ident", [128, 128])
    ltri_in = dram_in("ltri", [128, 128])
    anti_in = dram_in("anti", [128, 128])
    selh_in = dram_in("selh", [128, 2])
    NTH = NT // 2
    out_ext = nc.dram_tensor("out", [NTH, D], F32, kind="ExternalOutput")

    wsh_b = dram("wsh_b", [NR, 2048], BF16)
    wfull = dram("wfull", [8 * NR, 2048], BF16)
    eg_b = dram("eg_b", [D, FFE], BF16)
    eu_b = dram("eu_b", [D, FFE], BF16)
    ed_b = dram("ed_b", [FFE, D], BF16)
    xT = dram("xT", [D, NT], F32)
    x1T = dram("x1T", [D, NT], F32)
    oT = dram("oT", [D, NT], F32)
    qT = dram("qT", [D, NT], BF16)
    kv_loc_l = [dram(f"kv_loc{l}", [2 * NT, 2048], BF16) for l in range(L)]
    cpad = dram("cpad", [NH, CPW], BF16)
    Tdram = dram("Tdram", [NH * 128, TW], BF16)
    h2_loc = dram("h2_loc", [D, NT], BF16)
    h2_all = dram("h2_all", [8 * D, NT], BF16)
    gT_loc = dram("gT_loc", [8, NT], F32)
    gT_all = dram("gT_all", [64, NT], F32)
    yin = dram("yin", [8 * D, NT // 2], F32)
    yout = dram("yout", [D, NT // 2], F32)

    from contextlib import ExitStack
    es = ExitStack()
    with es:
        sbuf_names = set()

        def sb(name, shape, dt):
            sbuf_names.add(name)
            return es.enter_context(nc.sbuf_tensor(name, list(shape), dt))

        WPSZ = 4096
        A = sb("A", [128, KC, 512], F32)
        Bh = sb("Bh", [128, KC, 512], BF16)
        C = sb("C", [128, KC, 512], F32)
        WP = [sb(f"WP{i}", [128, WPSZ], BF16) for i in range(6)]
        ACTS = [sb(f"ACTS{i}", [128, 2, 512], BF16) for i in range(2)]
        QB = [sb(f"QB{i}", [128, 512], BF16) for i in range(2)]
        MASK = sb("MASK", [128, 8, 512], BF16)
        STG = [sb(f"STG{i}", [128, 512], BF16) for i in range(4)]
        STF = [sb(f"STF{i}", [128, 512], F32) for i in range(3)]
        W32 = [sb(f"W32{i}", [128, 512], F32) for i in range(4)]
        W16 = [sb(f"W16{i}", [128, 512], BF16) for i in range(6)]
        RSTD = [sb(f"RSTD{i}", [128, 512], F32) for i in range(2)]
        GVEC = sb("GVEC", [128, 5 * KC], F32)
        GMIX = sb("GMIX", [128, 2 * KC], F32)
        IDENT = sb("IDENT", [128, 128], F32)
        LTRI = sb("LTRI", [128, 128], BF16)
        NONES = sb("NONES", [128, 128], BF16)
        ANTI = sb("ANTI", [128, 128], BF16)
        ONES = sb("ONES", [128, 128], BF16)
        RELB = sb("RELB", [NBUCK, NH], F32)
        EXPB = sb("EXPB", [NBUCK, NH], F32)
        RW = sb("RW", [128, KC, 8], F32)
        SEL = sb("SEL", [8, 128], F32)
        SELH = sb("SELH", [128, 2], F32)
        LG = sb("LG", [8, 512], F32)
        GT = sb("GT", [8, 512], F32)
        SM = [sb(f"SM{i}", [128, 8], F32) for i in range(6)]
        SC = [sb(f"SC{i}", [128, 1], F32) for i in range(6)]
        PS = [es.enter_context(nc.psum_tensor(f"PS{i}", [128, 512], F32)) for i in range(8)]

        bufs = {}
        tname = {}

        def B(t):
            k = id(t)
            if k not in bufs:
                nm = getattr(t, "name", None)
                bufs[k] = Buf(nm if isinstance(nm, str) else str(k))
                tname[k] = bufs[k].name
            return bufs[k]

        sem_names = ["pe", "dve", "act", "pool", "cc"]

        def dma(q, out_ap, in_ap, rd_t, wr_t, key_t):
            sem = "d_" + B(key_t).name
            if sem not in sem_names:
                sem_names.append(sem)
            R.add(q, lambda e, o=out_ap, i=in_ap: e.dma_start(out=o, in_=i), [B(t) for t in rd_t],
                  [B(t) for t in wr_t], dma_sem=sem)

        def mm_group(out_ap, pairs, rd_t, wr_t):
            n = len(pairs)

            def fn(e, o=out_ap, pairs=pairs, n=n):
                ins = None
                for i, (l, r) in enumerate(pairs):
                    ins = e.matmul(o, l, r, start=(i == 0), stop=(i == n - 1))
                return ins
            R.add("pe", fn, [B(t) for t in rd_t], [B(t) for t in wr_t])

        def mm_acc(out_ap, l, r, start, stop, rd_t, wr_t):
            R.add("pe", lambda e: e.matmul(out_ap, l, r, start=start, stop=stop), [B(t) for t in rd_t],
                  [B(t) for t in wr_t])

        def act(out_ap, in_ap, func, rd_t, wr_t, bias=None, scale=None):
            kw = {}
            if bias is not None:
                kw["bias"] = bias
            if scale is not None:
                kw["scale"] = scale
            R.add("act", lambda e: e.activation(out_ap, in_ap, func, **kw), [B(t) for t in rd_t], [B(t) for t in wr_t])

        def tt(eng, out_ap, a, b, op, rd_t, wr_t):
            R.add(eng, lambda e: e.tensor_tensor(out_ap, a, b, op), [B(t) for t in rd_t], [B(t) for t in wr_t])

        def stt(out_ap, in0, scalar, in1, op0, op1, rd_t, wr_t, eng="dve"):
            R.add(eng, lambda e: e.scalar_tensor_tensor(out_ap, in0, scalar, in1, op0, op1), [B(t) for t in rd_t],
                  [B(t) for t in wr_t])

        def ts(out_ap, in0, s1, s2, op0, op1, rd_t, wr_t, eng="dve"):
            if op1 is None:
                R.add(eng, lambda e: e.tensor_single_scalar(out_ap, in0, s1, op0), [B(t) for t in rd_t],
                      [B(t) for t in wr_t])
            else:
                R.add(eng, lambda e: e.tensor_scalar(out_ap, in0, s1, s2, op0, op1), [B(t) for t in rd_t],
                      [B(t) for t in wr_t])

        def cp(eng, out_ap, in_ap, rd_t, wr_t):
            R.add(eng, lambda e: e.tensor_copy(out_ap, in_ap), [B(t) for t in rd_t], [B(t) for t in wr_t])

        def misc(eng, fn, rd_t, wr_t):
            R.add(eng, fn, [B(t) for t in rd_t], [B(t) for t in wr_t])

        def coll(kind, op, groups, in_t, out_t):
            R.add("pool", lambda e: e.collective_compute(kind, op, replica_groups=groups,
                                                         ins=[in_t.ap().opt()], outs=[out_t.ap().opt()]),
                  [B(in_t)], [B(out_t)], dma_sem="cc", inc=1)

        ALL8 = [list(range(8))]
        PAIRS = [[0, 1], [2, 3], [4, 5], [6, 7]]

        class Rot:
            def __init__(self, items):
                self.items = items; self.i = 0

            def get(self):
                it = self.items[self.i % len(self.items)]; self.i += 1
                return it
        ps_rot = Rot(PS[0:4])
        stg_rot = Rot(STG)
        stf_rot = Rot(STF)
        wp_rot = Rot(WP)

        def wp_load(src_ap, n1, n2, src_t, dshape=None):
            t = wp_rot.get()
            view = t[:, 0:n1 * n2].rearrange("p (a b) -> p a b", a=n1)
            if dshape is None:
                dma("sp", view, src_ap, [src_t], [t], t)
            else:
                for (dv_, sv_) in src_ap(t):
                    dma("sp", dv_, sv_, [src_t], [t], t)
            return view, t

        for (dst, src) in ((GVEC, gvec_in), (GMIX, gmix_in), (IDENT, ident_in), (RELB, relb_in), (SEL, sel_in), (SELH, selh_in)):
            dma("sp", dst[:], src[:, :], [], [dst], dst)
        dma("sp", RW[:], rw_in[:, :, :], [], [RW], RW)
        dma("pool", LTRI[:], ltri_in[:, :], [], [LTRI], LTRI)
        dma("pool", ANTI[:], anti_in[:, :], [], [ANTI], ANTI)
        dma("pool", MASK[:], mask_in.ap().rearrange("r p t -> p r t"), [], [MASK], MASK)
        misc("dve", lambda e: e.memset(ONES[:], 1.0), [], [ONES])
        misc("dve", lambda e: e.memset(NONES[:], -1.0), [], [NONES])

        for r0 in range(0, NR, 512):
            r1 = min(NR, r0 + 512)
            dma("pool", wsh_b[r0:r1, :], wsh_in[r0:r1, :], [], [wsh_b], wsh_b)
        coll("AllGather", ALU.bypass, ALL8, wsh_b, wfull)

        def wpanel_rows(name, rowlen, c0, ncols):
            def gen(t):
                v4 = t[:, 0:16 * ncols].rearrange("p (rk two c) -> p rk two c", rk=8, two=2)
                res = []
                for two in range(2):
                    off = off_rows[name] * 2048 + c0 + two * 128 * rowlen
                    res.append((v4[:, :, two, :], bass.AP(tensor=wfull, offset=off,
                                                          ap=[[rowlen, 128], [NRE, 8], [1, ncols]])))
                return res
            return gen

        def wpanel_dd(j0, gn):
            def gen(t):
                v4 = t[:, 0:gn * 2048].rearrange("p (j rk c) -> p j rk c", j=gn, rk=8)
                res = []
                for j in range(gn):
                    off = off_rows["dd"] * 2048 + (j0 + j) * 128 * 256
                    res.append((v4[:, j, :, :], bass.AP(tensor=wfull, offset=off, ap=[[256, 128], [NRE, 8], [1, 256]])))
                return res
            return gen

        if L >= 2 and not getattr(cfg, "nomoe", False):
            for (src, dstb) in ((eg_in, eg_b), (eu_in, eu_b), (ed_in, ed_b)):
                nrows = src.shape[0]
                for r0 in range(0, nrows, 512):
                    r1 = min(nrows, r0 + 512)
                    dma("pool", dstb[r0:r1, :], src[r0:r1, :], [], [dstb], dstb)

        act(EXPB[:], RELB[:], AF.Exp, [RELB], [EXPB])
        for ci, c0 in enumerate(range(0, CPW, 512)):
            c1 = min(CPW, c0 + 512)
            w = c1 - c0
            wtt = W32[ci % 2]
            dma("sp", wtt[0:NBUCK, 0:w], wt_in[:, c0:c1], [], [wtt], wtt)
            ps = ps_rot.get()
            mm_group(ps[0:NH, 0:w], [(EXPB[:, :], wtt[0:NBUCK, 0:w])], [EXPB, wtt], [ps])
            st = stg_rot.get()
            cp("dve", st[0:NH, 0:w], ps[0:NH, 0:w], [ps], [st])
            dma("sp", cpad[:, c0:c1], st[0:NH, 0:w], [st], [cpad], st)

        for h in range(NH):
            Trt = WP[h % 2]
            Trev = Trt[:, 0:TW]
            dma("sp", Trev, bass.AP(tensor=cpad, offset=h * CPW, ap=[[1, 128], [1, TW]]), [cpad], [Trt], Trt)
            for c0 in range(0, TW, 512):
                w = min(512, TW - c0)
                ps = ps_rot.get()
                mm_group(ps[:, 0:w], [(ANTI[:], Trev[:, c0:c0 + w])], [ANTI, Trt], [ps])
                st = stg_rot.get()
                cp("dve", st[:, 0:w], ps[:, 0:w], [ps], [st])
                dma("sp", Tdram[h * 128:(h + 1) * 128, c0:c0 + w], st[:, 0:w], [st], [Tdram], st)

        Crow = C[:, 0:4, :].rearrange("p k t -> p (k t)")
        for tb in range(cfg.NB):
            dma("sp", Crow, x_in[tb * 128:(tb + 1) * 128, :], [], [C], C)
            for k4 in range(4):
                ps = ps_rot.get()
                for j in range(4):
                    k = k4 * 4 + j
                    misc("pe", lambda e, ps=ps, j=j, k=k: e.transpose(ps[:, j * 128:(j + 1) * 128],
                                                                     Crow[:, k * 128:(k + 1) * 128], IDENT[:]),
                         [C, IDENT], [ps])
                cp("dve", A[:, k4 * 4:(k4 + 1) * 4, 0:128], ps[:].rearrange("p (j t) -> p j t", j=4), [ps], [A])
            dma("sp", xT.ap().rearrange("(k p) t -> p k t", p=128)[:, :, tb * 128:(tb + 1) * 128],
                A[:, :, 0:128], [A], [xT], A)

        def rms_rstd(src, chunks, nfeat, src_t, rstd_t):
            ps = PS[4]
            n = len(chunks)
            for i, k in enumerate(chunks):
                sq = W16[i % 2]
                act(sq[:], src[:, k, :], AF.Square, [src_t], [sq])
                mm_acc(ps[:], ONES[:], sq[:], i == 0, i == n - 1, [ONES, sq], [ps])
            ts(rstd_t[:], ps[:], 1.0 / nfeat, EPS, ALU.mult, ALU.add, [ps], [rstd_t])
            act(rstd_t[:], rstd_t[:], AF.Ln, [rstd_t], [rstd_t])
            act(rstd_t[:], rstd_t[:], AF.Exp, [rstd_t], [rstd_t], scale=-0.5)

        def xslot(t, s):
            return t.ap().rearrange("(k p) t -> p k t", p=128)[:, :, s * 512:(s + 1) * 512]

        def swiglu(wg, wu, wd, FF, into_A, dense=False):
            nch = FF // 128
            groups = [(j0, min(2, nch - j0)) for j0 in range(0, nch, 2)]
            for gi, (j0, gn) in enumerate(groups):
                if dense:
                    gv, gt_ = wp_load(wpanel_rows("dg", FF, j0 * 128, gn * 128), KC, gn * 128, wfull, (8, 2, gn * 128))
                    uv, ut_ = wp_load(wpanel_rows("du", FF, j0 * 128, gn * 128), KC, gn * 128, wfull, (8, 2, gn * 128))
                    dv, dt_ = wp_load(wpanel_dd(j0, gn), gn, D, wfull, (gn, 8, 256))
                else:
                    gv, gt_ = wp_load(wg.ap().rearrange("(k p) c -> p k c", p=128)[:, :, j0 * 128:(j0 + gn) * 128],
                                      KC, gn * 128, wg)
                    uv, ut_ = wp_load(wu.ap().rearrange("(k p) c -> p k c", p=128)[:, :, j0 * 128:(j0 + gn) * 128],
                                      KC, gn * 128, wu)
                    dv, dt_ = wp_load(wd.ap().rearrange("(j p) f -> p j f", p=128)[:, j0:j0 + gn, :], gn, D, wd)
                acts = ACTS[gi % 2]
                for c in range(gn):
                    pg = ps_rot.get()
                    mm_group(pg[:], [(gv[:, k, c * 128:(c + 1) * 128], Bh[:, k, :]) for k in range(KC)], [gt_, Bh], [pg])
                    pu = ps_rot.get()
                    mm_group(pu[:], [(uv[:, k, c * 128:(c + 1) * 128], Bh[:, k, :]) for k in range(KC)], [ut_, Bh], [pu])
                    sg = W32[c % 2]
                    act(sg[:], pg[:], AF.Silu, [pg], [sg])
                    tt("dve", acts[:, c, :], sg[:], pu[:], ALU.mult, [sg, pu], [acts])
                for f in range(KC):
                    py = PS[5 + f % 3]
                    mm_group(py[:], [(dv[:, c, f * 128:(f + 1) * 128], acts[:, c, :]) for c in range(gn)], [dt_, acts], [py])
                    if into_A:
                        tt("dve", A[:, f, :], A[:, f, :], py[:], ALU.add, [A, py], [A])
                    elif gi == 0:
                        cp("dve", C[:, f, :], py[:], [py], [C])
                    else:
                        tt("dve", C[:, f, :], C[:, f, :], py[:], ALU.add, [C, py], [C])

        for l in range(L):
            ag_col = l * KC
            fg_col = (2 + l) * KC
            kv_loc = kv_loc_l[l]
            kv_all = kv_loc
            for s in range(NS):
                dma("sp", A[:], xslot(xT, s), [xT], [A], A)
                rstd = RSTD[0]
                rms_rstd(A, list(range(KC)), D, A, rstd)
                for k in range(KC):
                    stt(Bh[:, k, :], A[:, k, :], GVEC[:, ag_col + k:ag_col + k + 1], rstd[:], ALU.mult, ALU.mult,
                        [A, GVEC, rstd], [Bh])
                for pnl in range(24):
                    pv, pt_ = wp_load(wpanel_rows(f"w_in{l}", 3 * D, pnl * 256, 256), KC, 256, wfull, (8, 2, 256))
                    kind = (pnl // 4) % 3
                    mixer = pnl // 12
                    hbase = mixer * 8 + (pnl % 4) * 2
                    if kind in (0, 1):
                        for c in range(2):
                            ps = ps_rot.get()
                            mm_group(ps[:], [(pv[:, k, c * 128:(c + 1) * 128], Bh[:, k, :]) for k in range(KC)],
                                     [pt_, Bh], [ps])
                            st = stg_rot.get()
                            if kind == 0:
                                act(st[:], ps[:], AF.Copy, [ps], [st], scale=float(HD ** -0.5))
                                dst = qT
                            else:
                                cp("dve", st[:], ps[:], [ps], [st])
                                dst = kv_loc
                            h = hbase + c
                            if kind == 0:
                                dap = qT[h * 128:(h + 1) * 128, s * 512:(s + 1) * 512]
                            else:
                                dap = bass.AP(tensor=kv_loc, offset=h * 128 * NT + s * 512, ap=[[NT, 128], [1, 512]])
                            dma("sp", dap, st[:], [st], [dst], st)
                    else:
                        for tb in range(4):
                            ps = ps_rot.get()
                            mm_group(ps[:, 0:256], [(Bh[:, k, tb * 128:(tb + 1) * 128], pv[:, k, :]) for k in range(KC)],
                                     [pt_, Bh], [ps])
                            st = stg_rot.get()
                            cp("dve", st[:, 0:256], ps[:, 0:256], [ps], [st])
                            c0 = hbase * 128
                            dma("sp", bass.AP(tensor=kv_loc, offset=NT * 2048 + (s * 512 + tb * 128) * 2048 + c0,
                                              ap=[[2048, 128], [1, 256]]), st[:, 0:256], [st], [kv_loc], st)
            l1s = getattr(cfg, "l1_stop", 99) if l == 1 else 99
            if l1s <= 2:
                break
            kv_i = 0
            for s in range(NS):
                nblk = 4 * s + 4
                for h in range(16):
                    slot = kv_i % 2; kv_i += 1
                    KTt = WP[slot * 2]; Vt = WP[slot * 2 + 1]
                    KT = KTt[:, 0:NT]
                    V = Vt[:, 0:NT].rearrange("p (l d) -> p l d", d=128)
                    dma("sp", KT[:, 0:nblk * 128],
                        bass.AP(tensor=kv_all, offset=h * 128 * NT, ap=[[NT, 128], [1, nblk * 128]]),
                        [kv_all], [KTt], KTt)
                    for j0 in range(0, nblk, 4):
                        dma("sp", V[:, j0:j0 + 4, :],
                            bass.AP(tensor=kv_all, offset=NT * 2048 + j0 * 128 * 2048 + h * 128,
                                    ap=[[2048, 128], [128 * 2048, 4], [1, 128]]), [kv_all], [Vt], Vt)
                    Q = QB[slot]
                    dma("sp", Q[:], qT[h * 128:(h + 1) * 128, s * 512:(s + 1) * 512], [qT], [Q], Q)
                    if h < 8:
                        Tt = WP[4 + h % 2]
                        T = Tt[:, 0:TW]
                        dma("sp", T, Tdram[h * 128:(h + 1) * 128, :], [Tdram], [Tt], Tt)
                        gs = list(range(max(0, 4 * s - 16), 4 * s + 4))
                        pnum, pden = PS[6], PS[7]
                        for gi, g in enumerate(gs):
                            pz = PS[gi % 2]
                            mm_group(pz[:], [(KT[:, g * 128:(g + 1) * 128], Q[:])], [KTt, Q], [pz])
                            E = W16[2 + gi % 2]
                            act(E[:], pz[:], AF.Exp, [pz], [E])
                            P = W16[4 + gi % 2]
                            J0 = 128 * (4 * s - g) + 896
                            tt("dve", P[:], E[:], T[:, J0:J0 + 512], ALU.mult, [E, Tt], [P])
                            mm_acc(pnum[:], V[:, g, :], P[:], gi == 0, gi == len(gs) - 1, [Vt, P], [pnum])
                            mm_acc(pden[:], ONES[:], P[:], gi == 0, gi == len(gs) - 1, [ONES, P], [pden])
                        rd = W32[2]
                        misc("dve", lambda e, rd=rd, pden=pden: e.reciprocal(rd[:], pden[:]), [pden], [rd])
                        of = stf_rot.get()
                        tt("dve", of[:], pnum[:], rd[:], ALU.mult, [pnum, rd], [of])
                    else:
                        gs = list(range(4 * s + 3, -1, -1))
                        po = PS[6]
                        Cacc = W32[3]
                        for gi, g in enumerate(gs):
                            kblk = KT[:, g * 128:(g + 1) * 128]
                            pz = PS[gi % 2]
                            mm_group(pz[:], [(kblk, Q[:])], [KTt, Q], [pz])
                            e32 = W32[gi % 2]
                            act(e32[:], pz[:], AF.Exp, [pz], [e32])
                            sp16 = W16[2 + gi % 2]
                            zone = g >= 4 * s
                            if zone:
                                act(e32[:], e32[:], AF.Ln, [e32], [e32], bias=1.0)
                                tt("dve", sp16[:], e32[:], MASK[:, g - 4 * s, :], ALU.mult, [e32, MASK], [sp16])
                            else:
                                act(sp16[:], e32[:], AF.Ln, [e32], [sp16], bias=1.0)
                            pa = PS[2 + gi % 2]
                            Cb = W16[4 + gi % 2]
                            pairs = [(kblk, Q[:]), (LTRI[:], sp16[:])]
                            rds = [KTt, Q, LTRI, sp16]
                            if gi > 0:
                                Cprev = W16[4 + (gi - 1) % 2]
                                pairs.append((NONES[:], Cprev[:]))
                                rds.append(Cprev)
                            mm_group(pa[:], pairs, rds, [pa])
                            Am = W16[gi % 2]
                            act(Am[:], pa[:], AF.Exp, [pa], [Am])
                            if zone:
                                tt("dve", Am[:], Am[:], MASK[:, g - 4 * s, :], ALU.mult, [Am, MASK], [Am])
                            if gi < len(gs) - 1:
                                if gi == 0:
                                    cp("pool", Cacc[:], sp16[:], [sp16], [Cacc])
                                else:
                                    tt("pool", Cacc[:], Cacc[:], sp16[:], ALU.add, [Cacc, sp16], [Cacc])
                                cp("pool", Cb[:], Cacc[:], [Cacc], [Cb])
                            mm_acc(po[:], V[:, g, :], Am[:], gi == 0, gi == len(gs) - 1, [Vt, Am], [po])
                        of = stf_rot.get()
                        cp("dve", of[:], po[:], [po], [of])
                    dma("sp", oT[h * 128:(h + 1) * 128, s * 512:(s + 1) * 512], of[:], [of], [oT], of)

            if l1s <= 3:
                break
            for s in range(NS):
                dma("sp", A[:], xslot(xT, s), [xT], [A], A)
                dma("sp", C[:], xslot(oT, s), [oT], [C], C)
                for m in range(2):
                    rstd = RSTD[m]
                    rms_rstd(C, list(range(8 * m, 8 * m + 8)), 1024, C, rstd)
                    for k in range(8 * m, 8 * m + 8):
                        stt(Bh[:, k, :], C[:, k, :], GMIX[:, l * KC + k:l * KC + k + 1], rstd[:], ALU.mult, ALU.mult,
                            [C, GMIX, rstd], [Bh])
                for pnl in range(8):
                    pv, pt_ = wp_load(wpanel_rows(f"w_o{l}", D, pnl * 256, 256), KC, 256, wfull, (8, 2, 256))
                    for c in range(2):
                        f = pnl * 2 + c
                        ps = ps_rot.get()
                        mm_group(ps[:], [(pv[:, k, c * 128:(c + 1) * 128], Bh[:, k, :]) for k in range(KC)], [pt_, Bh], [ps])
                        tt("dve", A[:, f, :], A[:, f, :], ps[:], ALU.add, [A, ps], [A])
                if l == L - 1:
                    dma("sp", xslot(x1T, s), A[:], [A], [x1T], A)
                rstd = RSTD[0]
                rms_rstd(A, list(range(KC)), D, A, rstd)
                for k in range(KC):
                    stt(Bh[:, k, :], A[:, k, :], GVEC[:, fg_col + k:fg_col + k + 1], rstd[:], ALU.mult, ALU.mult,
                        [A, GVEC, rstd], [Bh])
                if l % 2 == 0:
                    swiglu(None, None, None, FFD, True, dense=True)
                    dma("sp", xslot(xT, s), A[:], [A], [xT], A)
                elif getattr(cfg, "norouter", False):
                    dma("sp", xslot(h2_loc, s), Bh[:], [Bh], [h2_loc], Bh)
                else:
                    if s == 0:
                        for k in range(KC):
                            ts(RW[:, k, :], RW[:, k, :], GVEC[:, fg_col + k:fg_col + k + 1], None, ALU.mult, None,
                               [RW, GVEC], [RW])
                    pl = PS[5]
                    mm_group(pl[0:8, :], [(RW[:, k, :], A[:, k, :]) for k in range(KC)], [RW, A], [pl])
                    tt("dve", LG[:], pl[0:8, :], rstd[0:8, :], ALU.mult, [pl, rstd], [LG])
                    for tb in range(4):
                        pt = ps_rot.get()
                        mm_group(pt[:, 0:8], [(LG[:, tb * 128:(tb + 1) * 128], IDENT[0:8, 0:8])], [LG, IDENT], [pt])
                        lg, m1, eq1, lg2, m2, eq2 = SM[0], SC[0], SM[1], SM[2], SC[1], SM[3]
                        cp("dve", lg[:], pt[:, 0:8], [pt], [lg])
                        misc("dve", lambda e, m1=m1, lg=lg: e.tensor_reduce(m1[:], lg[:], AX.X, ALU.max), [lg], [m1])
                        ts(eq1[:], lg[:], m1[:, 0:1], None, ALU.is_equal, None, [lg, m1], [eq1])
                        stt(lg2[:], eq1[:], -1e30, lg[:], ALU.mult, ALU.add, [eq1, lg], [lg2])
                        misc("dve", lambda e, m2=m2, lg2=lg2: e.tensor_reduce(m2[:], lg2[:], AX.X, ALU.max), [lg2], [m2])
                        ts(eq2[:], lg2[:], m2[:, 0:1], None, ALU.is_equal, None, [lg2, m2], [eq2])
                        dd_, ee_, w1, w2 = SC[2], SC[3], SC[4], SC[5]
                        tt("dve", dd_[:], m2[:], m1[:], ALU.subtract, [m1, m2], [dd_])
                        act(ee_[:], dd_[:], AF.Exp, [dd_], [ee_])
                        ts(w1[:], ee_[:], 1.0, None, ALU.add, None, [ee_], [w1])
                        misc("dve", lambda e, w1=w1: e.reciprocal(w1[:], w1[:]), [w1], [w1])
                        tt("dve", w2[:], ee_[:], w1[:], ALU.mult, [ee_, w1], [w2])
                        gt = SM[4]
                        ts(gt[:], eq1[:], w1[:, 0:1], None, ALU.mult, None, [eq1, w1], [gt])
                        stt(gt[:], eq2[:], w2[:, 0:1], gt[:], ALU.mult, ALU.add, [eq2, w2, gt], [gt])
                        pg = ps_rot.get()
                        mm_group(pg[0:8, 0:128], [(gt[:, :], IDENT[:, :])], [gt, IDENT], [pg])
                        cp("dve", GT[:, tb * 128:(tb + 1) * 128], pg[0:8, 0:128], [pg], [GT])
                    dma("sp", gT_loc[:, s * 512:(s + 1) * 512], GT[:], [GT], [gT_loc], GT)
                    dma("sp", xslot(h2_loc, s), Bh[:], [Bh], [h2_loc], Bh)

        DO_MOE = L >= 2 and not getattr(cfg, "nomoe", False)
        if DO_MOE:
            coll("AllGather", ALU.bypass, ALL8, h2_loc, h2_all)
            coll("AllGather", ALU.bypass, ALL8, gT_loc, gT_all)
            GB = RSTD[1]
            HS = NS // 2
            for ti in range(4 * NS):
                bb, s = ti // NS, ti % NS
                r = 2 * bb
                dma("sp", Bh[:], h2_all.ap().rearrange("(r k p) t -> p r k t", r=8, p=128)[:, r, :, s * 512:(s + 1) * 512],
                    [h2_all], [Bh], Bh)
                dma("sp", GT[:], gT_all[r * 8:(r + 1) * 8, s * 512:(s + 1) * 512], [gT_all], [GT], GT)
                pgb = PS[4]
                mm_group(pgb[:], [(SEL[:, :], GT[:, :])], [SEL, GT], [pgb])
                act(GB[:], pgb[:], AF.Copy, [pgb], [GB])
                swiglu(eg_b, eu_b, ed_b, FFE, False)
                for f in range(KC):
                    tt("dve", C[:, f, :], C[:, f, :], GB[:], ALU.mult, [C, GB], [C])
                q = 2 * bb + s // HS
                dma("sp", yin.ap().rearrange("(q k p) t -> p q k t", q=8, p=128)[:, q, :, (s % HS) * 512:(s % HS + 1) * 512],
                    C[:], [C], [yin], C)
            coll("ReduceScatter", ALU.add, ALL8, yin, yout)
            res_src = x1T
        else:
            res_src = xT if (L < 2 or getattr(cfg, "l1_stop", 99) <= 3) else x1T

        fin_col = 4 * KC
        Arow = A[:, 0:4, :].rearrange("p k t -> p (k t)")
        for s in range(NS // 2):
            dma("sp", A[:], xslot(res_src, s), [res_src], [A], A)
            dma("sp", C[:], xslot(res_src, NS // 2 + s), [res_src], [C], C)
            for f in range(KC):
                ts(A[:, f, :], A[:, f, :], SELH[:, 0:1], None, ALU.mult, None, [A, SELH], [A])
                stt(A[:, f, :], C[:, f, :], SELH[:, 1:2], A[:, f, :], ALU.mult, ALU.add, [C, SELH, A], [A])
            if DO_MOE:
                dma("sp", C[:], xslot(yout, s), [yout], [C], C)
                for f in range(KC):
                    tt("dve", A[:, f, :], A[:, f, :], C[:, f, :], ALU.add, [A, C], [A])
            rstd = RSTD[0]
            rms_rstd(A, list(range(KC)), D, A, rstd)
            for k in range(KC):
                stt(C[:, k, :], A[:, k, :], GVEC[:, fin_col + k:fin_col + k + 1], rstd[:], ALU.mult, ALU.mult,
                    [A, GVEC, rstd], [C])
            for tb in range(4):
                for k4 in range(4):
                    ps = ps_rot.get()
                    for j in range(4):
                        k = k4 * 4 + j
                        misc("pe", lambda e, ps=ps, j=j, k=k, tb=tb: e.transpose(
                            ps[:, j * 128:(j + 1) * 128], C[:, k, tb * 128:(tb + 1) * 128], IDENT[:]), [C, IDENT], [ps])
                    cp("dve", Arow[:, k4 * 512:(k4 + 1) * 512], ps[:], [ps], [A])
                dma("sp", out_ext[s * 512 + tb * 128: s * 512 + (tb + 1) * 128, :], Arow, [A], [out_ext], A)

        R.finalize()
        sems = {}
        for n in sem_names:
            sems[n] = es.enter_context(nc.semaphore(n))
        block = es.enter_context(nc.Block())

        @block.tensor
        def _(e):
            R.emit("pe", e, sems)

        @block.vector
        def _(e):
            R.emit("dve", e, sems)

        @block.scalar
        def _(e):
            R.emit("act", e, sems)

        @block.gpsimd
        def _(e):
            R.emit("pool", e, sems)

        @block.sync
        def _(e):
            R.emit("sp", e, sems)
            for n in sem_names:
                if n.startswith("d_") or n == "cc":
                    if R.final.get(n, 0) > 0:
                        e.wait_ge(sems[n], R.final[n])
        print("ops:", len(R.ops), "sems:", len(sem_names), "final:", {k: v for k, v in sorted(R.final.items(), key=lambda kv: -kv[1])[:8]}, flush=True)
    return nc


def make_in_maps(cfg, x, rel_bias, attn_norm_g, w_in, mix_norm_dil_g, mix_norm_sb_g, w_o, ffn_norm_g,
                 dense_w_gate, dense_w_up, dense_w_down, router_w, moe_w_gate, moe_w_up, moe_w_down, final_norm_g):
    S, NT = cfg.S, cfg.NT
    f32 = np.float32
    col = lambda v: np.ascontiguousarray(np.asarray(v, f32).reshape(-1, 128).T)
    gv = [col(attn_norm_g[l]) for l in range(2)] + [col(ffn_norm_g[l]) for l in range(2)] + [col(final_norm_g)]
    gvec = np.ascontiguousarray(np.concatenate(gv, axis=1))
    gm = []
    for l in range(2):
        gm += [col(mix_norm_dil_g[l]), col(mix_norm_sb_g[l])]
    gmix = np.ascontiguousarray(np.concatenate(gm, axis=1))
    rw = np.ascontiguousarray(np.asarray(router_w[0], f32).reshape(KC, 128, 8).transpose(1, 0, 2))
    ident = np.eye(128, dtype=f32)
    ltri = -(np.arange(128)[:, None] >= np.arange(128)[None, :]).astype(f32)
    anti = np.ascontiguousarray(np.eye(128, dtype=f32)[::-1])
    maps = []
    for c in range(NCORE):
        b, par = c // 2, c % 2
        wt, mask = host_constants(0)
        sel = np.zeros((8, 128), f32); sel[c, :] = 1.0
        xc = np.ascontiguousarray(np.asarray(x[b], f32))
        selh = np.zeros((128, 2), f32); selh[:, par] = 1.0
        rows = slice(c * (D // 8), (c + 1) * (D // 8))
        maps.append({
            "x": xc,
            "wsh": np.ascontiguousarray(np.concatenate(
                [np.asarray(w[l], f32)[rows, :].reshape(-1, 2048) for l in range(cfg.depth) for w in (w_in, w_o)]
                + [np.asarray(dense_w_gate[0], f32)[rows, :].reshape(-1, 2048),
                   np.asarray(dense_w_up[0], f32)[rows, :].reshape(-1, 2048),
                   np.ascontiguousarray(np.asarray(dense_w_down[0], f32)[:, rows]).reshape(-1, 2048)], axis=0)),
            "eg": np.ascontiguousarray(np.asarray(moe_w_gate[0, c], f32)),
            "eu": np.ascontiguousarray(np.asarray(moe_w_up[0, c], f32)),
            "ed": np.ascontiguousarray(np.asarray(moe_w_down[0, c], f32)),
            "gvec": gvec, "gmix": gmix, "rel_bias": np.ascontiguousarray(np.asarray(rel_bias, f32)),
            "router_w": rw, "wt_c": wt, "mask_c": mask, "sel_c": sel, "selh": selh, "ident": ident, "ltri": ltri, "anti": anti,
        })
    return maps


def gather_out(cfg, results):
    S = cfg.S
    out = np.empty((4, S, D), np.float32)
    for c in range(NCORE):
        b, par = c // 2, c % 2
        out[b, par * (S // 2):(par + 1) * (S // 2)] = np.asarray(results[c]["out"])
    return out


_NC_CACHE = {}


def run(cfg, **inputs):
    key = (cfg.S, cfg.FFD, cfg.FFE, cfg.depth, getattr(cfg, "nomoe", False), getattr(cfg, "norouter", False), getattr(cfg, "l1_stop", 99))
    if key not in _NC_CACHE:
        _NC_CACHE[key] = build(cfg)
    nc = _NC_CACHE[key]
    maps = make_in_maps(cfg, **inputs)
    res = run_bass_kernel_spmd(nc, maps, core_ids=list(range(NCORE)))
    return gather_out(cfg, res.results)


def kernel(**inputs):
    return run(Cfg(), **inputs)
```
